# Optimizing a Trainium2 kernel written in Bass

```python
import math
import jax
import jax.numpy as jnp
from jax import lax
import numpy as np

D_MODEL = 2048
BATCH = 2
SEQ = 4096
DEPTH = 4

CTX_LEN = 256
GRID_W = 64
N_MIXERS = 3
EPS = 1e-6

SSM_EXPAND = 2
SSM_D_INNER = SSM_EXPAND * D_MODEL
SSM_HEAD_DIM = 64
SSM_HEADS = SSM_D_INNER // SSM_HEAD_DIM
SSM_GROUPS = 8
SSM_HPG = SSM_HEADS // SSM_GROUPS
SSM_STATE = 128
SSM_CONV = 5
SSM_CHUNK = 128
SSM_CONV_CH = SSM_D_INNER + 2 * SSM_GROUPS * SSM_STATE
SSM_IN_COLS = SSM_D_INNER + SSM_CONV_CH + 2 * SSM_HEADS
DT_MIN = 0.001
DT_MAX = 0.1

CV_CH = D_MODEL
CV_WIDTH = 31

SG_WIDTH = 2 * D_MODEL
SG_GROUPS = 8
SG_GROUP_CH = SG_WIDTH // SG_GROUPS
SG_CHUNK = 128
SG_CHUNK_ROWS = SG_CHUNK // GRID_W

PEER_HEADS = 8
PEER_N_KEYS = 128
PEER_EXPERTS = PEER_N_KEYS * PEER_N_KEYS
PEER_TOPK = 16
PEER_D_KEY = 256
PEER_D_HALF = PEER_D_KEY // 2
PEER_BLOCK = 128

N_SSM = (DEPTH + 2) // 3
N_CV = (DEPTH + 1) // 3
N_SG = DEPTH // 3

kernel_name = 'hybrid_ssd_conformer_gmlp_peer_dit'


def rmsnorm(x, g):
    xf = x.astype(jnp.float32)
    y = xf * lax.rsqrt(jnp.mean(xf * xf, axis=-1, keepdims=True) + EPS)
    return (y * g.astype(jnp.float32)).astype(x.dtype)


def layernorm(x, g, b):
    xf = x.astype(jnp.float32)
    mu = jnp.mean(xf, axis=-1, keepdims=True)
    var = jnp.mean(jnp.square(xf - mu), axis=-1, keepdims=True)
    y = (xf - mu) * lax.rsqrt(var + EPS)
    return (y * g.astype(jnp.float32) + b.astype(jnp.float32)).astype(x.dtype)


def modulate(h, shift, scale):
    return h * (1 + scale) + shift


def dwconv(x, w, b):
    k, ch = w.shape
    y = lax.conv_general_dilated(x, w.astype(x.dtype)[:, None, :], window_strides=(1,),
                                 padding=[(k // 2, k // 2)],
                                 dimension_numbers=('NWC', 'WIO', 'NWC'),
                                 feature_group_count=ch)
    return y + b.astype(x.dtype)


def _flip(t):
    return t[:, ::-1]


def ssd_chunked(x, dt, a_neg, bmat, cmat, init_state):
    bsz, seqlen = x.shape[:2]
    q = SSM_CHUNK
    nc = seqlen // q
    g, j, p, n = SSM_GROUPS, SSM_HPG, SSM_HEAD_DIM, SSM_STATE
    xd = (x * dt[..., None]).reshape(bsz, nc, q, g, j, p)
    a = (dt * a_neg).reshape(bsz, nc, q, g, j).transpose(0, 1, 3, 4, 2)
    a_cum = jnp.cumsum(a, axis=-1)
    bc = bmat.reshape(bsz, nc, q, g, n)
    cc = cmat.reshape(bsz, nc, q, g, n)
    lower = jnp.tril(jnp.ones((q, q), dtype=bool))
    seg = a_cum[..., :, None] - a_cum[..., None, :]
    decay_in = jnp.exp(jnp.where(lower, seg, -jnp.inf))
    cb = jnp.einsum('bclgn,bcsgn->bcgls', cc, bc)
    w_diag = cb[:, :, :, None] * decay_in
    y_diag = jnp.einsum('bcgjls,bcsgjp->bclgjp', w_diag, xd)
    decay_out = jnp.exp(a_cum[..., -1:] - a_cum).transpose(0, 1, 4, 2, 3)
    states = jnp.einsum('bcsgn,bcsgjp->bcgjpn', bc, xd * decay_out[..., None])
    chunk_decay = jnp.exp(a_cum[..., -1])

    def step(s, inp):
        st, dec = inp
        return s * dec[..., None, None] + st, s

    final, prev = lax.scan(step, init_state.reshape(bsz, g, j, p, n),
                           (jnp.moveaxis(states, 1, 0), jnp.moveaxis(chunk_decay, 1, 0)))
    prev = jnp.moveaxis(prev, 0, 1)
    cw = jnp.exp(a_cum).transpose(0, 1, 4, 2, 3)
    y_off = jnp.einsum('bclgn,bcgjpn->bclgjp', cc, prev) * cw[..., None]
    y = (y_diag + y_off).reshape(bsz, seqlen, g * j, p)
    return y, final.reshape(bsz, g * j, p, n)


def ssd_final_state(x, dt, a_neg, bmat):
    bsz, seqlen = x.shape[:2]
    a_cum = jnp.cumsum(dt * a_neg, axis=1)
    w = jnp.exp(a_cum[:, -1:] - a_cum) * dt
    xw = (x * w[..., None]).reshape(bsz, seqlen, SSM_GROUPS, SSM_HPG, SSM_HEAD_DIM)
    st = jnp.einsum('blgn,blgjp->bgjpn', bmat, xw)
    return st.reshape(bsz, SSM_HEADS, SSM_HEAD_DIM, SSM_STATE)


def ssm_inputs(h, w_in, conv_w, conv_b, dt_bias):
    bsz, seqlen = h.shape[:2]
    proj = h @ w_in
    z = proj[..., :SSM_D_INNER]
    xbc = jax.nn.silu(dwconv(proj[..., SSM_D_INNER:SSM_D_INNER + SSM_CONV_CH], conv_w, conv_b))
    dt_raw = proj[..., SSM_D_INNER + SSM_CONV_CH:]
    gn = SSM_GROUPS * SSM_STATE
    xs = xbc[..., :SSM_D_INNER].reshape(bsz, seqlen, SSM_HEADS, SSM_HEAD_DIM).astype(jnp.float32)
    bm = xbc[..., SSM_D_INNER:SSM_D_INNER + gn].reshape(bsz, seqlen, SSM_GROUPS, SSM_STATE).astype(jnp.float32)
    cm = xbc[..., SSM_D_INNER + gn:].reshape(bsz, seqlen, SSM_GROUPS, SSM_STATE).astype(jnp.float32)
    dt = jax.nn.softplus(dt_raw.astype(jnp.float32).reshape(bsz, seqlen, 2, SSM_HEADS)
                         + dt_bias.astype(jnp.float32))
    return z, xs, bm, cm, dt[:, :, 0], dt[:, :, 1]


def ssm_output(y, z, norm_g, w_out):
    bsz, seqlen = y.shape[:2]
    y = y.reshape(bsz, seqlen, SSM_D_INNER).astype(z.dtype) * jax.nn.silu(z)
    return rmsnorm(y, norm_g) @ w_out


def ssm_mixer(h_lat, h_ctx, w_in, conv_w, conv_b, dt_bias, a_log, d_skip, norm_g, w_out, need_ctx):
    a_neg = -jnp.exp(a_log.astype(jnp.float32))
    d_sum = (d_skip[0] + d_skip[1]).astype(jnp.float32)[:, None]
    zl, xl, bl, cl, dlf, dlb = ssm_inputs(h_lat, w_in, conv_w, conv_b, dt_bias)
    zc, xc, bc, cc, dcf, dcb = ssm_inputs(h_ctx, w_in, conv_w, conv_b, dt_bias)
    zero = jnp.zeros((h_ctx.shape[0], SSM_HEADS, SSM_HEAD_DIM, SSM_STATE), jnp.float32)
    y_ctx = None
    if need_ctx:
        yc_f, s_f = ssd_chunked(xc, dcf, a_neg[0], bc, cc, zero)
        yc_b, s_b = ssd_chunked(_flip(xc), _flip(dcb), a_neg[1], _flip(bc), _flip(cc), zero)
        y_ctx = ssm_output(yc_f + _flip(yc_b) + d_sum * xc, zc, norm_g, w_out)
    else:
        s_f = ssd_final_state(xc, dcf, a_neg[0], bc)
        s_b = ssd_final_state(_flip(xc), _flip(dcb), a_neg[1], _flip(bc))
    yl_f, _ = ssd_chunked(xl, dlf, a_neg[0], bl, cl, s_f)
    yl_b, _ = ssd_chunked(_flip(xl), _flip(dlb), a_neg[1], _flip(bl), _flip(cl), s_b)
    y_lat = ssm_output(yl_f + _flip(yl_b) + d_sum * xl, zl, norm_g, w_out)
    return y_lat, y_ctx


def conv_module(h, w1, b1, dw_w, dw_b, ln_g, ln_b, w2, b2):
    a = h @ w1 + b1
    glu = a[..., :CV_CH] * jax.nn.sigmoid(a[..., CV_CH:])
    d = dwconv(glu, dw_w, dw_b)
    return jax.nn.silu(layernorm(d, ln_g, ln_b)) @ w2 + b2


def chunk_gmlp(h, n_chunks, w_in, b_in, ln_g, ln_b, w_s, b_s, w_out, b_out):
    bsz, seqlen = h.shape[:2]
    zz = jax.nn.gelu(h @ w_in + b_in)
    u, v = zz[..., :SG_WIDTH], zz[..., SG_WIDTH:]
    v = layernorm(v, ln_g, ln_b).reshape(bsz, n_chunks, seqlen // n_chunks, SG_GROUPS, SG_GROUP_CH)
    mixed = jnp.einsum('gts,bcsgk->bctgk', w_s, v) + b_s.T[:, :, None]
    return (u * mixed.reshape(bsz, seqlen, SG_WIDTH)) @ w_out + b_out


def peer(xf, wq, keys, u_tab, v_tab):
    t_all, dm = xf.shape

    def block(xb):
        tb = xb.shape[0]
        q = (xb @ wq).reshape(tb, PEER_HEADS, 2, PEER_D_HALF)
        s1 = jnp.einsum('thd,kd->thk', q[:, :, 0], keys[0])
        s2 = jnp.einsum('thd,kd->thk', q[:, :, 1], keys[1])
        v1, i1 = lax.top_k(s1, PEER_TOPK)
        v2, i2 = lax.top_k(s2, PEER_TOPK)
        cand = (v1[..., :, None] + v2[..., None, :]).reshape(tb, PEER_HEADS, PEER_TOPK * PEER_TOPK)
        sv, ci = lax.top_k(cand, PEER_TOPK)
        e1 = jnp.take_along_axis(i1, ci // PEER_TOPK, axis=-1)
        e2 = jnp.take_along_axis(i2, ci % PEER_TOPK, axis=-1)
        idx = e1 * PEER_N_KEYS + e2
        gate = jax.nn.softmax(sv.astype(jnp.float32), axis=-1).astype(xb.dtype)
        u_sel = jnp.take(u_tab, idx, axis=0)
        v_sel = jnp.take(v_tab, idx, axis=0)
        act = jax.nn.gelu(jnp.einsum('thkd,td->thk', u_sel, xb))
        return jnp.einsum('thk,thkd->td', gate * act, v_sel)

    out = lax.map(block, xf.reshape(t_all // PEER_BLOCK, PEER_BLOCK, dm))
    return out.reshape(t_all, dm)


def setup_inputs(seed: int = 0) -> dict:
    key = jax.random.key(seed)
    ks = iter(jax.random.split(key, 64))
    f32 = jnp.float32
    D = D_MODEL

    def nrm(shape, scale):
        return jax.random.normal(next(ks), shape, f32) * scale

    def gain(shape):
        return 1.0 + nrm(shape, 0.02)

    dt0 = jnp.exp(jax.random.uniform(next(ks), (N_SSM, 2, SSM_HEADS), f32,
                                     math.log(DT_MIN), math.log(DT_MAX)))
    ssm_dt_bias = dt0 + jnp.log(-jnp.expm1(-dt0))
    ssm_a_log = jnp.log(jax.random.uniform(next(ks), (N_SSM, 2, SSM_HEADS), f32, 1.0, 16.0))
    return {
        'x': nrm((BATCH, SEQ, D), 1.0),
        'c': nrm((BATCH, D), 1.0),
        'ctx': nrm((BATCH, CTX_LEN, D), 1.0),
        'c_ctx': nrm((D,), 1.0),
        'w_mod': nrm((DEPTH, D, 6 * D), 0.5 * D ** -0.5),
        'b_mod': nrm((DEPTH, 6 * D), 0.02),
        'norm1_g': gain((DEPTH, D)),
        'norm2_g': gain((DEPTH, D)),
        'final_g': gain((D,)),
        'peer_wq': nrm((DEPTH, D, PEER_HEADS * PEER_D_KEY), D ** -0.5),
        'peer_keys': nrm((DEPTH, 2, PEER_N_KEYS, PEER_D_HALF), PEER_D_HALF ** -0.5),
        'peer_u': nrm((DEPTH, PEER_EXPERTS, D), D ** -0.5),
        'peer_v': nrm((DEPTH, PEER_EXPERTS, D), PEER_HEADS ** -0.5),
        'ssm_w_in': nrm((N_SSM, D, SSM_IN_COLS), D ** -0.5),
        'ssm_conv_w': nrm((N_SSM, SSM_CONV, SSM_CONV_CH), SSM_CONV ** -0.5),
        'ssm_conv_b': nrm((N_SSM, SSM_CONV_CH), 0.02),
        'ssm_dt_bias': ssm_dt_bias,
        'ssm_a_log': ssm_a_log,
        'ssm_d': 1.0 + nrm((N_SSM, 2, SSM_HEADS), 0.1),
        'ssm_norm_g': gain((N_SSM, SSM_D_INNER)),
        'ssm_w_out': nrm((N_SSM, SSM_D_INNER, D), SSM_D_INNER ** -0.5),
        'cv_w1': nrm((N_CV, D, 2 * CV_CH), D ** -0.5),
        'cv_b1': nrm((N_CV, 2 * CV_CH), 0.02),
        'cv_dw_w': nrm((N_CV, CV_WIDTH, CV_CH), CV_WIDTH ** -0.5),
        'cv_dw_b': nrm((N_CV, CV_CH), 0.02),
        'cv_ln_g': gain((N_CV, CV_CH)),
        'cv_ln_b': nrm((N_CV, CV_CH), 0.02),
        'cv_w2': nrm((N_CV, CV_CH, D), CV_CH ** -0.5),
        'cv_b2': nrm((N_CV, D), 0.02),
        'sg_w_in': nrm((N_SG, D, 2 * SG_WIDTH), D ** -0.5),
        'sg_b_in': nrm((N_SG, 2 * SG_WIDTH), 0.02),
        'sg_ln_g': gain((N_SG, SG_WIDTH)),
        'sg_ln_b': nrm((N_SG, SG_WIDTH), 0.02),
        'sg_w_s': nrm((N_SG, SG_GROUPS, SG_CHUNK, SG_CHUNK), SG_CHUNK ** -0.5),
        'sg_b_s': nrm((N_SG, SG_GROUPS, SG_CHUNK), 0.02),
        'sg_w_out': nrm((N_SG, SG_WIDTH, D), SG_WIDTH ** -0.5),
        'sg_b_out': nrm((N_SG, D), 0.02),
    }


def reference(x, c, ctx, c_ctx, w_mod, b_mod, norm1_g, norm2_g, final_g,
              peer_wq, peer_keys, peer_u, peer_v,
              ssm_w_in, ssm_conv_w, ssm_conv_b, ssm_dt_bias, ssm_a_log, ssm_d, ssm_norm_g, ssm_w_out,
              cv_w1, cv_b1, cv_dw_w, cv_dw_b, cv_ln_g, cv_ln_b, cv_w2, cv_b2,
              sg_w_in, sg_b_in, sg_ln_g, sg_ln_b, sg_w_s, sg_b_s, sg_w_out, sg_b_out):
    bsz, seq, dm = x.shape
    rows = seq // GRID_W
    lat_chunks = rows // SG_CHUNK_ROWS
    ctx_chunks = ctx.shape[1] // SG_CHUNK
    c_act = jax.nn.silu(c)
    cc_act = jax.nn.silu(c_ctx)
    for i in range(DEPTH):
        last = i == DEPTH - 1
        kind, j = i % N_MIXERS, i // N_MIXERS
        mod_l = (c_act @ w_mod[i] + b_mod[i])[:, None, :]
        mod_c = cc_act @ w_mod[i] + b_mod[i]
        sh1, sc1, g1, sh2, sc2, g2 = jnp.split(mod_l, 6, axis=-1)
        csh1, csc1, cg1, csh2, csc2, cg2 = jnp.split(mod_c, 6, axis=-1)
        hl = modulate(rmsnorm(x, norm1_g[i]), sh1, sc1)
        need_ctx = not last
        hc = modulate(rmsnorm(ctx, norm1_g[i]), csh1, csc1) if (need_ctx or kind == 0) else None
        if kind == 0:
            yl, yc = ssm_mixer(hl, hc, ssm_w_in[j], ssm_conv_w[j], ssm_conv_b[j], ssm_dt_bias[j],
                               ssm_a_log[j], ssm_d[j], ssm_norm_g[j], ssm_w_out[j], need_ctx)
        elif kind == 1:
            cv_p = (cv_w1[j], cv_b1[j], cv_dw_w[j], cv_dw_b[j], cv_ln_g[j], cv_ln_b[j], cv_w2[j], cv_b2[j])
            yl = conv_module(hl, *cv_p)
            yc = conv_module(hc, *cv_p) if need_ctx else None
        else:
            sg_p = (sg_w_in[j], sg_b_in[j], sg_ln_g[j], sg_ln_b[j], sg_w_s[j], sg_b_s[j], sg_w_out[j], sg_b_out[j])
            yl = chunk_gmlp(hl, lat_chunks, *sg_p)
            yc = chunk_gmlp(hc, ctx_chunks, *sg_p) if need_ctx else None
        x = x + g1 * yl
        hl2 = modulate(rmsnorm(x, norm2_g[i]), sh2, sc2)
        if need_ctx:
            ctx = ctx + cg1 * yc
            hc2 = modulate(rmsnorm(ctx, norm2_g[i]), csh2, csc2)
            n_lat = bsz * seq
            flat = jnp.concatenate([hl2.reshape(n_lat, dm), hc2.reshape(-1, dm)], axis=0)
            out = peer(flat, peer_wq[i], peer_keys[i], peer_u[i], peer_v[i])
            x = x + g2 * out[:n_lat].reshape(x.shape)
            ctx = ctx + cg2 * out[n_lat:].reshape(ctx.shape)
        else:
            out = peer(hl2.reshape(bsz * seq, dm), peer_wq[i], peer_keys[i], peer_u[i], peer_v[i])
            x = x + g2 * out.reshape(x.shape)
    return rmsnorm(x, final_g)
```

```python
import contextlib
import numpy as np
import ml_dtypes
import concourse.bass as bass
import concourse.mybir as mybir
from concourse.bass_utils import run_bass_kernel_spmd

F32 = mybir.dt.float32
BF16 = mybir.dt.bfloat16
AF = mybir.ActivationFunctionType
ALU = mybir.AluOpType
AX = mybir.AxisListType
NPBF = ml_dtypes.bfloat16

ENGS = ("pe", "act", "dve", "pool", "sp")

D = 2048
KC = D // 128
EPS = 1e-6
ARENA_WORDS = 52224


class Trk:
    __slots__ = ("name", "w", "r", "dsem", "excl")

    def __init__(self, name, excl=False):
        self.name = name
        self.w = None
        self.r = []
        self.dsem = None
        self.excl = excl


class Op:
    __slots__ = ("eng", "fn", "deps", "needs_inc", "is_dma", "sem", "val", "fuse")

    def __init__(self, eng, fn, is_dma=False):
        self.eng = eng
        self.fn = fn
        self.deps = []
        self.needs_inc = False
        self.is_dma = is_dma
        self.sem = None
        self.val = 0
        self.fuse = False


def _prod(xs):
    n = 1
    for x in xs:
        n *= int(x)
    return n


def _reshape(ap, shape):
    if len(shape) == 2:
        return ap
    if len(shape) == 3:
        return ap.rearrange("p (a b) -> p a b", a=shape[1])
    if len(shape) == 4:
        return ap.rearrange("p (a b c) -> p a b c", a=shape[1], b=shape[2])
    raise ValueError(shape)


class Prog:
    def __init__(self):
        self.nc = bass.Bass("TRN2", target_bir_lowering=False)
        self.es = contextlib.ExitStack()
        self.ops = {e: [] for e in ENGS}
        self.esem = {}
        self.dsems = []
        self.ntrk = 0
        self.out_ops = []
        self.arena = self.es.enter_context(self.nc.sbuf_tensor("arena", [128, ARENA_WORDS], F32))
        self.pbig = self.es.enter_context(self.nc.psum_tensor("pbig", [128, 4096], F32))
        self.sb_off = 0
        self.ps_off = 0
        self.free_slots = {"hw": [], "sw": []}
        self.phase_slots = []
        self.last_dma = {}

    def dram(self, name, shape, dt, kind):
        return self.nc.dram_tensor(name, list(shape), dt, kind=kind).ap()

    def din(self, name, shape, dt=F32):
        return self.dram(name, shape, dt, "ExternalInput")

    def dout(self, name, shape, dt=F32):
        return self.dram(name, shape, dt, "ExternalOutput")

    def dtmp(self, name, shape, dt=F32):
        return self.dram(name, shape, dt, "Internal")

    def sbuf(self, name, shape, dt=F32):
        n = _prod(shape[1:])
        words = n if dt == F32 else (n + 1) // 2
        words = (words + 7) // 8 * 8
        assert self.sb_off + words <= ARENA_WORDS, f"SBUF arena overflow at {name}: {self.sb_off}+{words}"
        ap = self.arena[0:shape[0], self.sb_off:self.sb_off + words]
        self.sb_off += words
        if dt != F32:
            ap = ap.bitcast(dt)
        ap = ap[:, 0:n]
        return _reshape(ap, shape)

    def psum(self, name, shape, dt=F32):
        n = _prod(shape[1:])
        nbytes = n * (4 if dt == F32 else 2)
        banks = (nbytes + 2047) // 2048
        assert self.ps_off + banks <= 8, f"PSUM overflow at {name}"
        ap = self.pbig[0:shape[0], self.ps_off * 512:(self.ps_off + banks) * 512]
        self.ps_off += banks
        if dt != F32:
            ap = ap.bitcast(dt)
        ap = ap[:, 0:n]
        return _reshape(ap, shape)

    def trk(self, name=None):
        self.ntrk += 1
        return Trk(name or f"t{self.ntrk}")

    def trks(self, n):
        return [self.trk() for _ in range(n)]

    def ptrk(self):
        self.ntrk += 1
        return Trk(f"p{self.ntrk}", excl=True)

    def ptrks(self, n):
        return [self.ptrk() for _ in range(n)]

    def _deps(self, o, r, w):
        deps = []
        seen = set()

        def add(d):
            if d is None or id(d) in seen:
                return
            if d.eng == "pe" and o.eng == "pe" and not d.is_dma and not o.is_dma:
                return
            seen.add(id(d))
            deps.append(d)

        w = list(w) + [t for t in r if t.excl]
        r = [t for t in r if not t.excl]
        for t in r:
            add(t.w)
        for t in w:
            add(t.w)
            for x in t.r:
                add(x)
        o.deps = deps
        for d in deps:
            d.needs_inc = True
        for t in r:
            t.r.append(o)
        for t in w:
            t.w = o
            t.r = []

    def op(self, eng, fn, r=(), w=(), fuse=False):
        o = Op(eng, fn)
        o.fuse = fuse
        self._deps(o, r, w)
        self.ops[eng].append(o)
        return o

    def dma(self, eng, out, in_, r=(), w=(), is_out=False, **kw):
        def fn(e):
            return e.dma_start(out=out, in_=in_, **kw)
        o = Op(eng, fn, is_dma=True)
        self._deps(o, r, w)
        key = w[0] if len(w) else r[0]
        kind = "sw" if eng == "pool" else "hw"
        assert eng in ("pool", "sp")
        if key.dsem is None:
            if self.free_slots[kind]:
                slot = self.free_slots[kind].pop()
            else:
                slot = len(self.dsems)
                self.dsems.append([None, 0, kind])
            key.dsem = slot
            self.phase_slots.append(slot)
        assert self.dsems[key.dsem][2] == kind, f"mixed DMA queue kinds on tracker {key.name}"
        o.sem = key.dsem
        self.last_dma[key.dsem] = o
        self.ops[eng].append(o)
        if is_out:
            self.out_ops.append(o)
        return o

    def barrier(self):
        deps = []
        for e in ENGS:
            for o in reversed(self.ops[e]):
                if not o.is_dma:
                    deps.append(o)
                    break
        for slot in sorted(set(self.phase_slots)):
            deps.append(self.last_dma[slot])
        for d in deps:
            d.needs_inc = True
        for e in ENGS:
            o = Op(e, lambda eng: eng.nop())
            o.deps = list(deps)
            self.ops[e].append(o)
        for slot in sorted(set(self.phase_slots)):
            self.free_slots[self.dsems[slot][2]].append(slot)
        self.phase_slots = []
        import os as _os
        if _os.environ.get("MEGA_DEBUG"):
            import sys as _sys
            import inspect as _insp
            tot = sum(len(v) for v in self.ops.values())
            waits = sum(len(o.deps) for v in self.ops.values() for o in v)
            prev = getattr(self, "_dbg_prev", (0, 0))
            print(f"[phase {_insp.stack()[1].function}] ops+={tot - prev[0]} deps+={waits - prev[1]} total={tot}", file=_sys.stderr)
            self._dbg_prev = (tot, waits)
        self.sb_off = 0
        self.ps_off = 0

    def mm(self, out, lhsT, rhs, start, stop, r=(), w=(), skip=False):
        if skip:
            return self.op("pe", lambda e: e.matmul(out, lhsT, rhs, start=start, stop=stop,
                                                    skip_group_check=True), r, w)
        return self.op("pe", lambda e: e.matmul(out, lhsT, rhs, start=start, stop=stop), r, w)

    def tr(self, out, in_, ident, r=(), w=()):
        return self.op("pe", lambda e: e.transpose(out, in_, ident), r, w)

    def act(self, out, in_, func, r=(), w=(), eng="act", **kw):
        return self.op(eng, lambda e: e.activation(out, in_, func, **kw), r, w, fuse=("accum_out" not in kw))

    def tt(self, eng, out, in0, in1, op, r=(), w=()):
        return self.op(eng, lambda e: e.tensor_tensor(out, in0, in1, op), r, w, fuse=True)

    def ts(self, eng, out, in0, s1, s2, op0, op1=ALU.bypass, r=(), w=(), **kw):
        return self.op(eng, lambda e: e.tensor_scalar(out, in0, s1, s2, op0, op1, **kw), r, w)

    def stt(self, eng, out, in0, scalar, in1, op0, op1, r=(), w=()):
        return self.op(eng, lambda e: e.scalar_tensor_tensor(out, in0, scalar, in1, op0, op1), r, w, fuse=True)

    def copy(self, eng, out, in_, r=(), w=()):
        if eng == "act":
            return self.op(eng, lambda e: e.copy(out, in_), r, w, fuse=True)
        return self.op(eng, lambda e: e.tensor_copy(out, in_), r, w, fuse=True)

    def memset(self, eng, ap, val, w=()):
        return self.op(eng, lambda e: e.memset(ap, val), (), w)

    def build(self):
        nc = self.nc
        es = self.es
        for e in ENGS:
            self.esem[e] = es.enter_context(nc.semaphore(f"sem_{e}"))
        for i, d in enumerate(self.dsems):
            d[0] = es.enter_context(nc.semaphore(f"dsem{i}"))
        for e in ENGS:
            c = 0
            for o in self.ops[e]:
                if o.is_dma:
                    d = self.dsems[o.sem]
                    d[1] += 16
                    o.sem = d[0]
                    o.val = d[1]
                elif o.needs_inc:
                    c += 1
                    o.sem = self.esem[e]
                    o.val = c
        out_ops = self.out_ops
        ops = self.ops

        def emit(eng_name, eng):
            waited = {}
            for o in ops[eng_name]:
                need = []
                for d in o.deps:
                    k = id(d.sem)
                    if waited.get(k, 0) >= d.val:
                        continue
                    waited[k] = d.val
                    need.append(d)
                last = need.pop() if (o.fuse and need) else None
                for d in need:
                    eng.wait_ge(d.sem, d.val)
                ins = o.fn(eng)
                if last is not None:
                    ins._wait_ge(last.sem, last.val)
                if o.is_dma:
                    ins.then_inc(o.sem, 16)
                elif o.needs_inc:
                    ins.then_inc(o.sem, 1)
            if eng_name == "sp":
                for o in out_ops:
                    k = id(o.sem)
                    if waited.get(k, 0) >= o.val:
                        continue
                    waited[k] = o.val
                    eng.wait_ge(o.sem, o.val)

        with nc.Block() as block:
            @block.tensor
            def _(e):
                emit("pe", e)

            @block.scalar
            def _(e):
                emit("act", e)

            @block.vector
            def _(e):
                emit("dve", e)

            @block.gpsimd
            def _(e):
                emit("pool", e)

            @block.sync
            def _(e):
                emit("sp", e)
        es.close()
        return nc


N_LAUNCH = [0]


def run(nc, in_maps, tag=""):
    import sys
    import time
    N_LAUNCH[0] += 1
    t0 = time.time()
    res = run_bass_kernel_spmd(nc, in_maps, core_ids=list(range(len(in_maps))))
    nbytes = sum(v.nbytes for m in in_maps for v in m.values())
    print(f"[launch {N_LAUNCH[0]} {tag}] {time.time() - t0:.1f}s in={nbytes / 1e6:.0f}MB", file=sys.stderr, flush=True)
    return res.results


IDENT = np.eye(128, dtype=np.float32)
NEG_BIG = -1.0e30
GELU_C0 = 0.044715
GELU_C1 = 1.5957691216057308
THR_MARGIN = 2.0e-5


def emit_gelu(P, dst, xb, sqb, t_x, t_sq, t_dst, n):
    P.act(sqb[:, :n], xb[:, :n], AF.Square, r=[t_x], w=[t_sq])
    P.ts("dve", sqb[:, :n], sqb[:, :n], GELU_C0, 1.0, ALU.mult, ALU.add, r=[t_sq], w=[t_sq])
    P.tt("dve", sqb[:, :n], sqb[:, :n], xb[:, :n], ALU.mult, r=[t_sq, t_x], w=[t_sq])
    P.act(sqb[:, :n], sqb[:, :n], AF.Sigmoid, r=[t_sq], w=[t_sq], scale=GELU_C1)
    P.tt("dve", dst, sqb[:, :n], xb[:, :n], ALU.mult, r=[t_sq, t_x], w=[t_dst])


def emit_mod(P, cTd, wd, bd, modd, nl):
    cT = P.sbuf("cT_sb", [128, KC, 2])
    t_cT = P.trk()
    wsb = [P.sbuf(f"w_sb{i}", [128, KC, 768]) for i in range(2)]
    t_w = P.trks(2)
    bsb = [P.sbuf(f"b_sb{i}", [2, 768]) for i in range(2)]
    t_b = P.trks(2)
    osb = [P.sbuf(f"o_sb{i}", [2, 768]) for i in range(2)]
    t_o = P.trks(2)
    ps = [P.psum(f"ps{i}", [128, 512]) for i in range(2)]
    t_ps = P.ptrks(2)
    P.dma("sp", cT, cTd, w=[t_cT])
    P.act(cT, cT, AF.Silu, r=[t_cT], w=[t_cT])
    n = 0
    for l in range(nl):
        wv = wd[l].rearrange("(kc p) n -> p kc n", p=128)
        for cb in range(16):
            k = n % 2
            n += 1
            cs = slice(cb * 768, (cb + 1) * 768)
            for q in range(0, KC, 4):
                P.dma("sp", wsb[k][:, q:q + 4, :], wv[:, q:q + 4, cs], w=[t_w[k]])
            P.dma("sp", bsb[k], bd[l, cs].partition_broadcast(2), w=[t_b[k]])
            for j, (c0, c1) in enumerate(((0, 512), (512, 768))):
                m = c1 - c0
                for kc in range(KC):
                    P.mm(ps[j][0:2, :m], cT[:, kc, :], wsb[k][:, kc, c0:c1], kc == 0, kc == KC - 1,
                         r=[t_cT, t_w[k]], w=[t_ps[j]])
                P.tt("dve", osb[k][:, c0:c1], ps[j][0:2, :m], bsb[k][:, c0:c1], ALU.add,
                     r=[t_ps[j], t_b[k]], w=[t_o[k]])
            P.dma("sp", modd[l, :, cs], osb[k], r=[t_o[k]])
    P.barrier()


def emit_prep(P, tiles, x_src, x_dst, part, rows, ty, ident, hT_out=None, out_dst=None, d=D, plain=False):
    kc_n = d // 128
    nhalf = d // 1024
    final = out_dst is not None
    used_rows = [3] if plain else list(range(8))
    rb = P.sbuf("rows_bc", [128, len(used_rows), d])
    ri = {r: i for i, r in enumerate(used_rows)}
    t_rb = P.trk()
    idb = P.sbuf("ident_bf", [128, 128], BF16)
    t_id = P.trk()
    xb = [P.sbuf(f"xb{i}", [128, d]) for i in range(2)]
    t_xb = P.trks(2)
    if part is not None:
        yb = [P.sbuf(f"yb{i}", [128, d]) for i in range(2)]
        t_yb = P.trks(2)
    st = P.sbuf("stat", [128, 4])
    t_st = P.trk()
    hf = P.sbuf("hf", [128, d])
    t_hf = P.trk()
    junk, t_junk = hf, t_hf
    if not final:
        hb = [P.sbuf(f"hb{i}", [128, d], BF16) for i in range(2)]
        t_hb = P.trks(2)
        hTs = [P.sbuf(f"hTs{i}", [128, kc_n, 128], BF16) for i in range(2)]
        t_hTs = P.trks(2)
        pT = [P.psum(f"pT{i}", [128, 1024], BF16) for i in range(2)]
        t_pT = P.ptrks(2)
    t_xd = P.trks(4)

    for r in used_rows:
        if rows[r] is None:
            P.memset("pool", rb[:, ri[r], :], 0.0, w=[t_rb])
        else:
            P.dma("sp", rb[:, ri[r], :], rows[r].partition_broadcast(128), w=[t_rb])
    P.dma("pool", idb, ident, w=[t_id])
    if not plain:
        for k in range(2):
            P.stt("dve", rb[:, 4 + k, :], rb[:, 4 + k, :], 1.0, rb[:, 3, :], ALU.add, ALU.mult,
                  r=[t_rb], w=[t_rb])

    npt = 0
    for n, t in enumerate(tiles):
        k = ty(t)
        b = n % 2
        xt = xb[b]
        P.dma("sp", xt, x_src(t), r=[t_xd[n % 4]], w=[t_xb[b]])
        if part is not None:
            P.dma("sp", yb[b], part(t), w=[t_yb[b]])
            P.tt("dve", yb[b], yb[b], rb[:, 2, :], ALU.add, r=[t_yb[b], t_rb], w=[t_yb[b]])
            P.tt("dve", yb[b], yb[b], rb[:, k, :], ALU.mult, r=[t_yb[b], t_rb], w=[t_yb[b]])
            P.tt("dve", xt, xt, yb[b], ALU.add, r=[t_yb[b], t_xb[b]], w=[t_xb[b]])
        if x_dst is not None:
            P.dma("sp", x_dst(t), xt, r=[t_xb[b]], w=[t_xd[n % 4]])
        P.act(junk, xt, AF.Square, r=[t_xb[b]], w=[t_junk, t_st], accum_out=st[:, 0:1])
        P.ts("dve", st[:, 1:2], st[:, 0:1], 1.0 / d, EPS, ALU.mult, ALU.add, r=[t_st], w=[t_st])
        P.act(st[:, 2:3], st[:, 1:2], AF.Sqrt, r=[t_st], w=[t_st])
        P.op("dve", lambda e: e.reciprocal(st[:, 3:4], st[:, 2:3]), r=[t_st], w=[t_st])
        if plain:
            P.stt("dve", hb[b], xt, st[:, 3:4], rb[:, 0, :], ALU.mult, ALU.mult,
                  r=[t_xb[b], t_st, t_rb], w=[t_hb[b]])
        else:
            P.stt("dve", hf, xt, st[:, 3:4], rb[:, 4 + k, :], ALU.mult, ALU.mult,
                  r=[t_xb[b], t_st, t_rb], w=[t_hf])
            if final:
                P.tt("dve", xt, hf, rb[:, 6 + k, :], ALU.add, r=[t_hf, t_rb], w=[t_xb[b]])
                P.dma("sp", out_dst(t), xt, r=[t_xb[b]], is_out=True)
                continue
            P.tt("dve", hb[b], hf, rb[:, 6 + k, :], ALU.add, r=[t_hf, t_rb], w=[t_hb[b]])
        for half in range(nhalf):
            pp = npt % 2
            npt += 1
            for k8 in range(8):
                c = half * 8 + k8
                P.tr(pT[pp][:, k8 * 128:(k8 + 1) * 128], hb[b][:, c * 128:(c + 1) * 128], idb,
                     r=[t_hb[b], t_id], w=[t_pT[pp]])
            P.copy("act", hTs[b][:, half * 8:(half + 1) * 8, :],
                   pT[pp].rearrange("p (k t) -> p k t", k=8), r=[t_pT[pp]], w=[t_hTs[b]])
        P.dma("sp", hT_out[:, :, t * 128:(t + 1) * 128], hTs[b], r=[t_hTs[b]])
    P.barrier()


def emit_lin(P, nt, xTd, wd, yd):
    kc_n = xTd.shape[1]
    TB = 8
    xs = P.sbuf("xT_sb", [128, kc_n, TB * 128], BF16)
    t_xs = P.trk()
    ws = [P.sbuf(f"w_sb{i}", [128, kc_n, 512], BF16) for i in range(2)]
    t_ws = P.trks(2)
    osb = [P.sbuf(f"osb{i}", [128, 512]) for i in range(2)]
    t_osb = P.trks(2)
    o_ps = [P.psum(f"o_ps{i}", [128, 512]) for i in range(2)]
    t_ops = P.ptrks(2)
    wv = wd.rearrange("(kc p) n -> p kc n", p=128)
    n = 0
    nw = 0
    for t0 in range(0, nt, TB):
        ntb = min(TB, nt - t0)
        P.dma("sp", xs[:, :, 0:ntb * 128], xTd[:, :, t0 * 128:(t0 + ntb) * 128], w=[t_xs])
        for nb in range(4):
            wb = nw % 2
            nw += 1
            for q in range(0, kc_n, 4):
                P.dma("pool", ws[wb][:, q:q + 4, :], wv[:, q:q + 4, nb * 512:(nb + 1) * 512], w=[t_ws[wb]])
            for tl in range(ntb):
                t = t0 + tl
                k = n % 2
                n += 1
                for kc in range(kc_n):
                    P.mm(o_ps[k], xs[:, kc, tl * 128:(tl + 1) * 128], ws[wb][:, kc, :], kc == 0, kc == kc_n - 1,
                         r=[t_xs, t_ws[wb]], w=[t_ops[k]])
                P.copy("act" if k else "dve", osb[k], o_ps[k], r=[t_ops[k]], w=[t_osb[k]])
                P.dma("sp", yd[t * 128:(t + 1) * 128, nb * 512:(nb + 1) * 512], osb[k], r=[t_osb[k]])
    P.barrier()


def ssd_consts():
    k = np.arange(128)[:, None]
    s = np.arange(128)[None, :]
    c = np.zeros((2, 128, 640), np.float32)
    c[0, :, 0:128] = (k > s)
    c[0, :, 128:256] = 1.0
    c[0, :, 256:384] = (k <= s)
    c[0, :, 384:512] = (k <= s)
    c[1, :, 0:128] = (k < s)
    c[1, :, 128:256] = 1.0
    c[1, :, 256:384] = (k >= s)
    c[1, :, 384:512] = (k >= s)
    c[:, :, 512:640] = np.eye(128, dtype=np.float32)
    return c


def emit_ssd_group(P, hTd, wzd, wxd, wdd, cwd, cbd, vecd, cstd, y_dst, yfd, ctx_g, lat_g):
    nch = len(ctx_g) + len(lat_g)
    t_yfd = P.trk()
    wz = P.sbuf("wz_sb", [128, KC, 512], BF16)
    wx = P.sbuf("wx_sb", [128, KC, 768], BF16)
    wd = P.sbuf("wd_sb", [128, KC, 16], BF16)
    t_w = P.trk()
    cw = P.sbuf("cw_sb", [128, 6, 5])
    cb = P.sbuf("cb_sb", [128, 6])
    vec = P.sbuf("vec_sb", [128, 3, 16])
    cst = P.sbuf("cst_sb", [128, 2, 640])
    t_c = P.trk()
    wbc = P.sbuf("wbc", [128, 5, 6, 128])
    bbc = P.sbuf("bbc", [128, 6, 128])
    t_wbc = P.trk()
    aneg = P.sbuf("aneg", [128, 16])
    dsum = P.sbuf("dsum", [128, 8])
    t_an = P.trk()
    X = P.sbuf("X", [128, nch, 512], BF16)
    BTM = P.sbuf("BTM", [128, nch, 128], BF16)
    BCT = P.sbuf("BCT", [128, nch, 256], BF16)
    DTA = P.sbuf("DTA", [128, nch, 32])
    t_st = P.trks(nch)
    hTx = [P.sbuf(f"hTx{i}", [128, KC, 132], BF16) for i in range(2)]
    t_hTx = P.trks(2)
    hTz = [P.sbuf(f"hTz{i}", [128, KC, 128], BF16) for i in range(2)]
    t_hTz = P.trks(2)
    acc = [P.sbuf(f"cacc{i}", [128, 3, 128]) for i in range(2)]
    t_acc = P.trks(2)
    post = P.sbuf("post", [128, 6, 128])
    t_post = P.trks(2)
    dtw = P.sbuf("dtw", [128, 16])
    t_dtw = P.trk()
    xd = [P.sbuf(f"xd{i}", [128, 512], BF16) for i in range(2)]
    xdd = [P.sbuf(f"xdd{i}", [128, 512], BF16) for i in range(2)]
    t_xd = P.trks(2)
    dec = [P.sbuf(f"dec{i}", [128, 16]) for i in range(2)]
    t_dec = P.trks(2)
    CC = [P.sbuf(f"CC{i}", [128, 256]) for i in range(2)]
    t_CC = P.trks(2)
    L2 = [P.sbuf(f"L2_{i}", [128, 256]) for i in range(2)]
    t_L2 = P.trks(2)
    E2 = [P.sbuf(f"E2_{i}", [128, 256]) for i in range(2)]
    t_E2 = P.trks(2)
    LC = [P.sbuf(f"LC{i}", [128, 256], BF16) for i in range(2)]
    t_LC = P.trks(2)
    Sst = [P.sbuf(f"Sst{i}", [128, 512]) for i in range(2)]
    Sbf = [P.sbuf(f"Sbf{i}", [128, 512], BF16) for i in range(2)]
    t_S = P.trks(2)
    t_Sbf = P.trks(2)
    ysb = [P.sbuf(f"ysb{i}", [128, 512]) for i in range(2)]
    t_ysb = P.trks(2)
    y2 = [P.sbuf(f"y2_{i}", [128, 512]) for i in range(2)]
    t_y2 = P.trks(2)
    zs = P.sbuf("zs", [128, 512])
    t_zs = P.trk()

    pre_full = [P.psum(f"pre_ps{i}", [128, 512]) for i in range(2)]
    pre_ps = [p[:, 0:396].rearrange("p (c t) -> p c t", c=3) for p in pre_full]
    t_pre = P.ptrks(2)
    tp_ps = P.psum("tp_ps", [128, 512])
    t_tp = P.ptrk()
    mi_ps = P.psum("mi_ps", [128, 512])
    t_cbp = t_dtp = t_dep = t_btp = P.ptrk()
    sg_ps = [P.psum(f"sg_ps{i}", [128, 512]) for i in range(2)]
    t_sg = P.ptrks(2)
    y_ps = P.psum("y_ps", [128, 512])
    t_yps = P.ptrk()
    st_ps = P.psum("st_ps", [128, 512])
    t_stp = P.ptrk()

    wzv = wzd.rearrange("(kc p) n -> p kc n", p=128)
    wxv = wxd.rearrange("(kc p) n -> p kc n", p=128)
    for i in range(4):
        P.dma("pool", wz[:, i * 4:(i + 1) * 4, :], wzv[:, i * 4:(i + 1) * 4, :], w=[t_w])
        P.dma("pool", wx[:, i * 4:(i + 1) * 4, :], wxv[:, i * 4:(i + 1) * 4, :], w=[t_w])
    P.dma("pool", wd, wdd.rearrange("(kc p) n -> p kc n", p=128), w=[t_w])
    P.dma("sp", cw, cwd, w=[t_c])
    P.dma("sp", cb, cbd, w=[t_c])
    for i in range(3):
        P.dma("sp", vec[:, i, :], vecd[i].partition_broadcast(128), w=[t_c])
    P.dma("sp", cst, cstd.rearrange("a p n -> p a n"), w=[t_c])
    ones = cst[:, 0, 128:256]
    identf = cst[:, 0, 512:640]
    for k in range(5):
        for ch in range(6):
            P.ts("dve", wbc[:, k, ch, :], ones, cw[:, ch, k:k + 1], None, ALU.mult, r=[t_c], w=[t_wbc])
    for ch in range(6):
        P.ts("dve", bbc[:, ch, :], ones, cb[:, ch:ch + 1], None, ALU.mult, r=[t_c], w=[t_wbc])
    P.act(aneg, vec[:, 1, :], AF.Exp, r=[t_c], w=[t_an])
    P.ts("dve", aneg, aneg, -1.0, None, ALU.mult, r=[t_an], w=[t_an])
    P.tt("dve", dsum, vec[:, 2, 0:8], vec[:, 2, 8:16], ALU.add, r=[t_c], w=[t_an])
    for i in range(2):
        P.memset("pool", Sst[i], 0.0, w=[t_S[i]])
        P.memset("pool", Sbf[i], 0.0, w=[t_Sbf[i]])

    cnt = {"hx": 0, "hd": 0}

    def hcols(g, c0, c1):
        return hTd[:, :, g * 128 + c0:g * 128 + c1]

    def prep_chunk(ci, gid, gprev, gnext):
        b = cnt["hx"] % 2
        cnt["hx"] += 1
        H = hTx[b]
        P.dma("sp", H[:, :, 2:130], hcols(gid, 0, 128), w=[t_hTx[b]])
        if gprev is None:
            P.memset("pool", H[:, :, 0:2], 0.0, w=[t_hTx[b]])
        else:
            P.dma("sp", H[:, :, 0:2], hcols(gprev, 126, 128), w=[t_hTx[b]])
        if gnext is None:
            P.memset("pool", H[:, :, 130:132], 0.0, w=[t_hTx[b]])
        else:
            P.dma("sp", H[:, :, 130:132], hcols(gnext, 0, 2), w=[t_hTx[b]])
        for hf in range(2):
            for c3 in range(3):
                ch = hf * 3 + c3
                for kc in range(KC):
                    P.mm(pre_ps[hf][:, c3, :], wx[:, kc, ch * 128:(ch + 1) * 128], H[:, kc, :],
                         kc == 0, kc == KC - 1, r=[t_w, t_hTx[b]], w=[t_pre[hf]])
            A = acc[hf]
            chs = slice(hf * 3, hf * 3 + 3)
            P.tt("dve", A, pre_ps[hf][:, :, 0:128], wbc[:, 0, chs, :], ALU.mult, r=[t_pre[hf], t_wbc], w=[t_acc[hf]])
            P.tt("dve", A, A, bbc[:, chs, :], ALU.add, r=[t_acc[hf], t_wbc], w=[t_acc[hf]])
            for k in range(1, 5):
                P.tt("dve", post[:, chs, :], pre_ps[hf][:, :, k:k + 128], wbc[:, k, chs, :], ALU.mult,
                     r=[t_pre[hf], t_wbc], w=[t_post[hf]])
                P.tt("dve", A, A, post[:, chs, :], ALU.add, r=[t_acc[hf], t_post[hf]], w=[t_acc[hf]])
            P.act(post[:, chs, :], A, AF.Silu, r=[t_acc[hf]], w=[t_post[hf]])
        for kc in range(KC):
            P.mm(mi_ps[:, 128:144], H[:, kc, 2:130], wd[:, kc, :], kc == 0, kc == KC - 1,
                 r=[t_w, t_hTx[b]], w=[t_dtp])
        P.tt("dve", dtw, mi_ps[:, 128:144], vec[:, 0, :], ALU.add, r=[t_dtp, t_c], w=[t_dtw])
        P.act(dtw, dtw, AF.Exp, r=[t_dtw], w=[t_dtw])
        P.act(DTA[:, ci, 0:16], dtw, AF.Ln, r=[t_dtw], w=[t_st[ci]], bias=1.0)
        P.tt("dve", DTA[:, ci, 16:32], DTA[:, ci, 0:16], aneg, ALU.mult, r=[t_st[ci], t_an], w=[t_st[ci]])
        for c4 in range(4):
            P.tr(tp_ps[:, c4 * 128:(c4 + 1) * 128], post[:, c4, :], identf, r=[t_post[0], t_post[1], t_c], w=[t_tp])
        P.copy("act", X[:, ci, :], tp_ps, r=[t_tp], w=[t_st[ci]])
        P.tr(mi_ps[:, 160:288], post[:, 4, :], identf, r=[t_post[1], t_c], w=[t_btp])
        P.copy("act", BTM[:, ci, :], mi_ps[:, 160:288], r=[t_btp], w=[t_st[ci]])
        P.copy("pool", BCT[:, ci, :].rearrange("p (a t) -> p a t", a=2), post[:, 4:6, :], r=[t_post[1]], w=[t_st[ci]])

    def ssd_step(ci, d, gid, final):
        k = cnt["hd"] % 2
        dt_d = DTA[:, ci, d * 8:(d + 1) * 8]
        a_d = DTA[:, ci, 16 + d * 8:16 + (d + 1) * 8]
        ML_ones = cst[:, d, 0:256]
        R = cst[:, d, 256:384]
        Mc = cst[:, d, 384:512]
        rs = [t_st[ci]]
        x3 = X[:, ci, :].rearrange("p (h q) -> p h q", h=8)
        xd3 = xd[k].rearrange("p (h q) -> p h q", h=8)
        P.tt("dve", xd3, x3, dt_d.unsqueeze(2).to_broadcast([128, 8, 64]), ALU.mult, r=rs, w=[t_xd[k]])
        P.mm(mi_ps[:, 144:152], cst[:, d, 0:128], a_d, True, True, r=rs + [t_c], w=[t_dep])
        P.mm(mi_ps[:, 152:160], ones, a_d, True, True, r=rs + [t_c], w=[t_dep])
        P.act(dec[k], mi_ps[:, 144:160], AF.Exp, r=[t_dep], w=[t_dec[k]])
        P.tt("dve", xdd[k].rearrange("p (h q) -> p h q", h=8), xd3,
             dec[k][:, 0:8].unsqueeze(2).to_broadcast([128, 8, 64]), ALU.mult, r=[t_xd[k], t_dec[k]], w=[t_xd[k]])
        P.mm(mi_ps[:, 0:128], BCT[:, ci, 0:128], BCT[:, ci, 128:256], True, True, r=rs, w=[t_cbp])
        P.tt("dve", CC[k][:, 0:128], mi_ps[:, 0:128], Mc, ALU.mult, r=[t_cbp, t_c], w=[t_CC[k]])
        P.copy("pool", CC[k][:, 128:256], BCT[:, ci, 128:256], r=rs, w=[t_CC[k]])
        for h in range(8):
            j = (cnt["hd"] * 8 + h) % 2
            P.ts("dve", L2[j], ML_ones, a_d[:, h:h + 1], None, ALU.mult, r=rs + [t_c], w=[t_L2[j]])
            P.mm(sg_ps[j][:, 0:128], L2[j][:, 0:128], R, True, True, r=[t_L2[j], t_c], w=[t_sg[j]])
            P.mm(sg_ps[j][:, 128:256], L2[j][:, 128:256], R, True, True, r=[t_L2[j], t_c], w=[t_sg[j]])
            P.act(E2[j], sg_ps[j][:, 0:256], AF.Exp, r=[t_sg[j]], w=[t_E2[j]])
            P.tt("dve", LC[j], E2[j], CC[k], ALU.mult, r=[t_E2[j], t_CC[k]], w=[t_LC[j]])
            hs = slice(h * 64, (h + 1) * 64)
            P.mm(y_ps[:, hs], LC[j][:, 0:128], xd[k][:, hs], True, False, r=[t_LC[j], t_xd[k]], w=[t_yps])
            P.mm(y_ps[:, hs], LC[j][:, 128:256], Sbf[d][:, hs], False, True, r=[t_LC[j], t_Sbf[d]], w=[t_yps])
            P.mm(st_ps[:, hs], BTM[:, ci, :], xdd[k][:, hs], True, True, r=rs + [t_xd[k]], w=[t_stp])
        S3 = Sst[d].rearrange("p (h q) -> p h q", h=8)
        P.tt("dve", S3, S3, dec[k][:, 8:16].unsqueeze(2).to_broadcast([128, 8, 64]), ALU.mult,
             r=[t_S[d], t_dec[k]], w=[t_S[d]])
        P.tt("dve", Sst[d], Sst[d], st_ps, ALU.add, r=[t_S[d], t_stp], w=[t_S[d]])
        P.copy("act", Sbf[d], Sst[d], r=[t_S[d]], w=[t_Sbf[d]])
        sl = slice(gid * 128, (gid + 1) * 128)
        if not final:
            P.tt("dve", ysb[k].rearrange("p (h q) -> p h q", h=8), x3,
                 dsum.unsqueeze(2).to_broadcast([128, 8, 64]), ALU.mult, r=rs + [t_an], w=[t_ysb[k]])
            P.tt("dve", ysb[k], ysb[k], y_ps, ALU.add, r=[t_ysb[k], t_yps], w=[t_ysb[k]])
            P.dma("sp", yfd[sl, :], ysb[k], r=[t_ysb[k]], w=[t_yfd])
        else:
            zb = cnt["hd"] % 2
            P.dma("sp", hTz[zb], hcols(gid, 0, 128), w=[t_hTz[zb]])
            P.dma("sp", ysb[k], yfd[sl, :], r=[t_yfd], w=[t_ysb[k]])
            for kc in range(KC):
                P.mm(tp_ps, hTz[zb][:, kc, :], wz[:, kc, :], kc == 0, kc == KC - 1, r=[t_hTz[zb], t_w], w=[t_tp])
            P.act(zs, tp_ps, AF.Silu, r=[t_tp], w=[t_zs])
            P.tt("dve", y2[k], ysb[k], y_ps, ALU.add, r=[t_ysb[k], t_yps], w=[t_y2[k]])
            P.tt("dve", y2[k], y2[k], zs, ALU.mult, r=[t_y2[k], t_zs], w=[t_y2[k]])
            P.dma("sp", y_dst(gid), y2[k], r=[t_y2[k]])
        cnt["hd"] += 1

    seq = list(ctx_g) + list(lat_g)
    nctx = len(ctx_g)
    for ci, gid in enumerate(seq):
        first = ci == 0 or ci == nctx
        last = ci == nctx - 1 or ci == len(seq) - 1
        prep_chunk(ci, gid, None if first else seq[ci - 1], None if last else seq[ci + 1])
        ssd_step(ci, 0, gid, False)
    order = list(range(nctx - 1, -1, -1)) + list(range(len(seq) - 1, nctx - 1, -1))
    for ci in order:
        ssd_step(ci, 1, seq[ci], True)
    P.barrier()


CV_COLS = 1056 + 160


def emit_conv_slab(P, hTd, s, w1d, vecd, dwd, onesd, sTd, n_lat, n_ctx):
    CW = CV_COLS
    TO = 1152
    hT = P.sbuf("hT_sb", [128, KC, CW], BF16)
    t_hT = P.trk()
    mk = P.sbuf("mask_bc", [128, CW])
    t_mk = P.trk()
    vec = P.sbuf("vec_sb", [128, 4, 32])
    dww = P.sbuf("dww_sb", [128, KC, 31])
    ones = P.sbuf("ones_sb", [128, 128])
    t_c = P.trk()
    w1s = [P.sbuf(f"w1s{i}", [128, KC, 256], BF16) for i in range(2)]
    t_w1 = P.trks(2)
    sig = [P.sbuf(f"sig{i}", [128, 512]) for i in range(2)]
    t_sig = P.trks(2)
    glu = [P.sbuf(f"glu{i}", [128, CW]) for i in range(2)]
    t_glu = P.trks(2)
    dT = P.sbuf("dT", [128, KC, TO])
    t_dT = P.trks(KC)
    sq = [P.sbuf(f"sq{i}", [128, TO]) for i in range(2)]
    t_sq = P.trks(2)
    mean = P.sbuf("mean", [128, TO])
    rstd = P.sbuf("rstd", [128, TO])
    t_mr = P.trk()
    tmp = sq
    t_tmp = t_sq
    sT = hT[:, :, 0:TO]
    v_ps = P.psum("v_ps", [128, 512])
    g_ps = P.psum("g_ps", [128, 512])
    t_vps = P.ptrk()
    t_gps = P.ptrk()
    sum_ps = [P.psum(f"sum_ps{i}", [128, 512]) for i in range(3)]
    ssq_ps = [P.psum(f"ssq_ps{i}", [128, 512]) for i in range(3)]
    t_sum = P.ptrks(3)
    t_ssq = P.ptrks(3)

    P.memset("pool", hT, 0.0, w=[t_hT])
    P.memset("pool", mk, 0.0, w=[t_mk])
    L = n_lat * 128
    lo, hi = s * 1024 - 16, s * 1024 + 1040
    a0, a1 = max(lo, 0), min(hi, L)
    P.dma("sp", hT[:, :, a0 - lo:a1 - lo], hTd[:, :, a0:a1], w=[t_hT])
    P.memset("pool", mk[:, a0 - lo:a1 - lo], 1.0, w=[t_mk])
    k = s % n_ctx
    Lc = n_ctx * 128
    lo, hi = k * 128 - 16, k * 128 + 144
    a0, a1 = max(lo, 0), min(hi, Lc)
    P.dma("sp", hT[:, :, 1056 + a0 - lo:1056 + a1 - lo], hTd[:, :, L + a0:L + a1], w=[t_hT])
    P.memset("pool", mk[:, 1056 + a0 - lo:1056 + a1 - lo], 1.0, w=[t_mk])
    P.dma("sp", vec, vecd, w=[t_c])
    P.dma("sp", dww, dwd, w=[t_c])
    P.dma("sp", ones, onesd, w=[t_c])
    w1v = w1d.rearrange("(kc p) n -> p kc n", p=128)
    cblocks = [(0, 512), (512, 1024), (1024, CW)]
    sblocks = [(0, 512), (512, 1024), (1024, TO)]
    for v in range(KC):
        wb = v % 2
        P.dma("pool", w1s[wb][:, :, 0:128], w1v[:, :, v * 128:(v + 1) * 128], w=[t_w1[wb]])
        P.dma("pool", w1s[wb][:, :, 128:256], w1v[:, :, D + v * 128:D + (v + 1) * 128], w=[t_w1[wb]])
        G = glu[wb]
        for bi, (c0, c1) in enumerate(cblocks):
            n = c1 - c0
            sb_ = (v * 3 + bi) % 2
            for kc in range(KC):
                P.mm(v_ps[:, :n], w1s[wb][:, kc, 0:128], hT[:, kc, c0:c1], kc == 0, kc == KC - 1,
                     r=[t_w1[wb], t_hT], w=[t_vps])
            for kc in range(KC):
                P.mm(g_ps[:, :n], w1s[wb][:, kc, 128:256], hT[:, kc, c0:c1], kc == 0, kc == KC - 1,
                     r=[t_w1[wb], t_hT], w=[t_gps])
            P.act(sig[sb_][:, :n], g_ps[:, :n], AF.Sigmoid, r=[t_gps, t_c], w=[t_sig[sb_]],
                  bias=vec[:, 0, 16 + v:17 + v])
            P.stt("dve", G[:, c0:c1], v_ps[:, :n], vec[:, 0, v:v + 1], sig[sb_][:, :n], ALU.add, ALU.mult,
                  r=[t_vps, t_sig[sb_], t_c], w=[t_glu[wb]])
        P.tt("pool", G, G, mk, ALU.mult, r=[t_glu[wb], t_mk], w=[t_glu[wb]])
        for (o0, n, base) in ((0, 1024, 0), (1024, 128, 1056)):
            dst = dT[:, v, o0:o0 + n]
            P.ts("dve", dst, G[:, base + 1:base + 1 + n], dww[:, v, 0:1], vec[:, 1, v:v + 1], ALU.mult, ALU.add,
                 r=[t_glu[wb], t_c], w=[t_dT[v]])
            for kk in range(1, 31):
                P.stt("dve", dst, G[:, base + kk + 1:base + kk + 1 + n], dww[:, v, kk:kk + 1], dst, ALU.mult, ALU.add,
                      r=[t_glu[wb], t_c, t_dT[v]], w=[t_dT[v]])
        P.act(sq[wb], dT[:, v, :], AF.Square, r=[t_dT[v]], w=[t_sq[wb]])
        for bi, (c0, c1) in enumerate(sblocks):
            n = c1 - c0
            P.mm(sum_ps[bi][:, :n], ones, dT[:, v, c0:c1], v == 0, v == KC - 1, r=[t_c, t_dT[v]], w=[t_sum[bi]])
            P.mm(ssq_ps[bi][:, :n], ones, sq[wb][:, c0:c1], v == 0, v == KC - 1, r=[t_c, t_sq[wb]], w=[t_ssq[bi]])
    for bi, (c0, c1) in enumerate(sblocks):
        n = c1 - c0
        P.ts("dve", mean[:, c0:c1], sum_ps[bi][:, :n], 1.0 / D, None, ALU.mult, r=[t_sum[bi]], w=[t_mr])
        P.ts("dve", rstd[:, c0:c1], ssq_ps[bi][:, :n], 1.0 / D, EPS, ALU.mult, ALU.add, r=[t_ssq[bi]], w=[t_mr])
    P.tt("dve", tmp[0], mean, mean, ALU.mult, r=[t_mr], w=[t_tmp[0]])
    P.tt("dve", rstd, rstd, tmp[0], ALU.subtract, r=[t_mr, t_tmp[0]], w=[t_mr])
    P.act(rstd, rstd, AF.Sqrt, r=[t_mr], w=[t_mr])
    P.op("dve", lambda e: e.reciprocal(rstd, rstd), r=[t_mr], w=[t_mr])
    for v in range(KC):
        kk = v % 2
        P.tt("dve", tmp[kk], dT[:, v, :], mean, ALU.subtract, r=[t_dT[v], t_mr], w=[t_tmp[kk]])
        P.tt("pool", tmp[kk], tmp[kk], rstd, ALU.mult, r=[t_tmp[kk], t_mr], w=[t_tmp[kk]])
        P.act(sT[:, v, :], tmp[kk], AF.Silu, r=[t_tmp[kk], t_c], w=[t_hT],
              scale=vec[:, 2, v:v + 1], bias=vec[:, 3, v:v + 1])
    nl = min(1024, L - s * 1024)
    P.dma("sp", sTd[:, :, s * 1024:s * 1024 + nl], sT[:, :, 0:nl], r=[t_hT])
    P.dma("sp", sTd[:, :, L + k * 128:L + (k + 1) * 128], sT[:, :, 1024:1152], r=[t_hT])
    P.barrier()


def conv_inputs(w1, b1, dw_w, dw_b, ln_g, ln_b):
    vec = np.zeros((128, 4, 32), np.float32)
    vec[:, 0, :] = b1.reshape(32, 128).T
    vec[:, 1, :16] = dw_b.reshape(16, 128).T
    vec[:, 2, :16] = ln_g.reshape(16, 128).T
    vec[:, 3, :16] = ln_b.reshape(16, 128).T
    return vec, np.ascontiguousarray(dw_w.reshape(31, 16, 128).transpose(2, 1, 0))


def emit_gmlp(P, nt, hTd, wind, bind, lnd, wsd, bsd, ident, gTd, grp=3):
    W = 4096
    hT = P.sbuf("hT_sb", [128, KC, grp * 128], BF16)
    t_hT = P.trk()
    lnb = P.sbuf("ln_bc", [128, 2, W])
    wsT = P.sbuf("wsT_sb", [128, 8, 128], BF16)
    bsT = P.sbuf("bsT_sb", [128, 8])
    idb = P.sbuf("ident_bf", [128, 128], BF16)
    t_c = P.trk()
    t_c2 = P.trk()
    ws = [P.sbuf(f"w_sb{i}", [128, KC, 512], BF16) for i in range(2)]
    t_ws = P.trks(2)
    bb = [P.sbuf(f"b_bc{i}", [128, 512]) for i in range(2)]
    t_bb = P.trks(2)
    zu = P.sbuf("zu", [128, grp, W], BF16)
    zv = P.sbuf("zv", [128, grp, W])
    t_z = P.trks(grp)
    xb = [P.sbuf(f"xb{i}", [128, 512]) for i in range(2)]
    t_xb = P.trks(2)
    sqb = [P.sbuf(f"sqb{i}", [128, 512]) for i in range(2)]
    t_sqb = P.trks(2)
    st = P.sbuf("stat", [128, 8])
    t_st = P.trk()
    vbf = P.sbuf("vbf", [128, W], BF16)
    t_vbf = P.trk()
    gat = P.sbuf("gated", [128, W], BF16)
    t_gat = P.trk()
    junk, t_junk = gat, t_gat
    gTs = [P.sbuf(f"gTs{i}", [128, 32, 128], BF16) for i in range(2)]
    t_gTs = P.trks(2)
    z_ps = [P.psum(f"z_ps{i}", [128, 512]) for i in range(2)]
    t_zps = P.ptrks(2)
    m_ps = [P.psum(f"m_ps{i}", [128, 512]) for i in range(2)]
    t_mps = P.ptrks(2)
    pT = [P.psum(f"pT{i}", [128, 1024], BF16) for i in range(2)]
    t_pT = P.ptrks(2)

    for i in range(2):
        P.dma("sp", lnb[:, i, :], lnd[i].partition_broadcast(128), w=[t_c])
    P.dma("pool", wsT, wsd.rearrange("g s t -> s g t"), w=[t_c2])
    P.dma("sp", bsT, bsd, w=[t_c])
    P.dma("pool", idb, ident, w=[t_c2])
    wv = wind.rearrange("(kc p) n -> p kc n", p=128)
    nw = 0
    nz = 0
    npt = 0
    for t0 in range(0, nt, grp):
        ng = min(grp, nt - t0)
        P.dma("sp", hT[:, :, 0:ng * 128], hTd[:, :, t0 * 128:(t0 + ng) * 128], w=[t_hT])
        for nb in range(16):
            wb = nw % 2
            nw += 1
            for q in range(0, KC, 4):
                P.dma("pool", ws[wb][:, q:q + 4, :], wv[:, q:q + 4, nb * 512:(nb + 1) * 512], w=[t_ws[wb]])
            P.dma("sp", bb[wb], bind[0, nb * 512:(nb + 1) * 512].partition_broadcast(128), w=[t_bb[wb]])
            for tl in range(ng):
                k = nz % 2
                nz += 1
                for kc in range(KC):
                    P.mm(z_ps[k], hT[:, kc, tl * 128:(tl + 1) * 128], ws[wb][:, kc, :], kc == 0, kc == KC - 1,
                         r=[t_hT, t_ws[wb]], w=[t_zps[k]])
                P.tt("dve", xb[k], z_ps[k], bb[wb], ALU.add, r=[t_zps[k], t_bb[wb]], w=[t_xb[k]])
                if nb < 8:
                    dst = zu[:, tl, nb * 512:(nb + 1) * 512]
                else:
                    dst = zv[:, tl, (nb - 8) * 512:(nb - 7) * 512]
                emit_gelu(P, dst, xb[k], sqb[k], t_xb[k], t_sqb[k], t_z[tl], 512)
        for tl in range(ng):
            t = t0 + tl
            V = zv[:, tl, :]
            P.op("dve", lambda e, V=V: e.tensor_reduce(st[:, 0:1], V, AX.X, ALU.add), r=[t_z[tl]], w=[t_st])
            P.act(junk, V, AF.Square, r=[t_z[tl]], w=[t_junk, t_st], accum_out=st[:, 1:2])
            P.ts("dve", st[:, 2:3], st[:, 0:1], 1.0 / W, None, ALU.mult, r=[t_st], w=[t_st])
            P.tt("dve", st[:, 3:4], st[:, 2:3], st[:, 2:3], ALU.mult, r=[t_st], w=[t_st])
            P.ts("dve", st[:, 4:5], st[:, 1:2], 1.0 / W, EPS, ALU.mult, ALU.add, r=[t_st], w=[t_st])
            P.tt("dve", st[:, 4:5], st[:, 4:5], st[:, 3:4], ALU.subtract, r=[t_st], w=[t_st])
            P.act(st[:, 5:6], st[:, 4:5], AF.Sqrt, r=[t_st], w=[t_st])
            P.op("dve", lambda e: e.reciprocal(st[:, 6:7], st[:, 5:6]), r=[t_st], w=[t_st])
            P.ts("dve", V, V, st[:, 2:3], st[:, 6:7], ALU.subtract, ALU.mult, r=[t_z[tl], t_st], w=[t_z[tl]])
            P.tt("pool", V, V, lnb[:, 0, :], ALU.mult, r=[t_z[tl], t_c], w=[t_z[tl]])
            P.tt("dve", vbf, V, lnb[:, 1, :], ALU.add, r=[t_z[tl], t_c], w=[t_vbf])
            for g in range(8):
                k = g % 2
                gs = slice(g * 512, (g + 1) * 512)
                P.mm(m_ps[k], wsT[:, g, :], vbf[:, gs], True, True, r=[t_c2, t_vbf], w=[t_mps[k]])
                P.stt("dve", gat[:, gs], m_ps[k], bsT[:, g:g + 1], zu[:, tl, gs], ALU.add, ALU.mult,
                      r=[t_mps[k], t_c, t_z[tl]], w=[t_gat])
            b = t % 2
            for half in range(4):
                pp = npt % 2
                npt += 1
                for k8 in range(8):
                    c = half * 8 + k8
                    P.tr(pT[pp][:, k8 * 128:(k8 + 1) * 128], gat[:, c * 128:(c + 1) * 128], idb,
                         r=[t_gat, t_c2], w=[t_pT[pp]])
                P.copy("act", gTs[b][:, half * 8:(half + 1) * 8, :],
                       pT[pp].rearrange("p (k t) -> p k t", k=8), r=[t_pT[pp]], w=[t_gTs[b]])
            P.dma("sp", gTd[:, :, t * 128:(t + 1) * 128], gTs[b], r=[t_gTs[b]])
    P.barrier()


def emit_peer1(P, nt, hTd, wq, keysT, s1d, s2d, thd):
    hT = P.sbuf("hT_sb", [128, KC, 512], BF16)
    t_hT = P.trk()
    wqs = P.sbuf("wq_sb", [128, KC, D], BF16)
    t_wq = P.trks(4)
    kts = P.sbuf("keysT_sb", [128, 2, 128])
    t_kt = P.trk()
    qT = [P.sbuf(f"qT{i}", [128, 512]) for i in range(2)]
    t_qT = P.trks(2)
    ssb = [P.sbuf(f"s_sb{i}", [128, 2, 8, 128]) for i in range(4)]
    t_ssb = P.trks(4)
    q_ps = [P.psum(f"q_ps{i}", [128, 512]) for i in range(2)]
    t_qps = P.ptrks(2)
    s_ps = [P.psum(f"s_ps{i}", [128, 512]) for i in range(2)]
    t_sps = P.ptrks(2)
    v16 = P.sbuf("v16", [128, 2, 16])
    t_v16 = P.trk()
    tmp = P.sbuf("tmp128", [128, 128])
    t_tmp = P.trk()
    cand = P.sbuf("cand", [128, 256])
    t_cand = P.trk()
    cand2 = P.sbuf("cand2", [128, 256])
    t_cand2 = P.trk()
    top = [P.sbuf(f"top{i}", [128, 8, 16]) for i in range(2)]
    t_top = P.trks(2)
    e16 = P.sbuf("e16", [128, 16])
    t_e16 = P.trk()
    sm = [P.sbuf(f"sm{i}", [128, 6, 8]) for i in range(2)]
    t_sm = P.trks(2)
    s2m = [P.sbuf(f"s2m{i}", [128, 8, 128]) for i in range(2)]
    t_s2m = P.trks(2)

    wq_v = wq.rearrange("(kc p) n -> p kc n", p=128)
    for i in range(4):
        P.dma("pool", wqs[:, i * 4:(i + 1) * 4, :], wq_v[:, i * 4:(i + 1) * 4, :], w=[t_wq[i]])
    P.dma("sp", kts, keysT.rearrange("a d k -> d a k"), w=[t_kt])

    nq = 0
    ns = 0
    for b0 in range(0, nt, 4):
        ntb = min(4, nt - b0)
        bw = ntb * 128
        P.dma("sp", hT[:, :, 0:bw], hTd[:, :, b0 * 128:b0 * 128 + bw], w=[t_hT])
        for n in range(16):
            h, half = n // 2, n % 2
            qp = nq % 2
            nq += 1
            for kc in range(KC):
                P.mm(q_ps[qp][:, :bw], wqs[:, kc, n * 128:(n + 1) * 128], hT[:, kc, 0:bw],
                     kc == 0, kc == KC - 1, r=[t_wq[kc // 4], t_hT], w=[t_qps[qp]])
            P.copy("act", qT[qp][:, :bw], q_ps[qp][:, :bw], r=[t_qps[qp]], w=[t_qT[qp]])
            for tl in range(ntb):
                sp_ = ns % 2
                ns += 1
                P.mm(s_ps[sp_][:, 0:128], qT[qp][:, tl * 128:(tl + 1) * 128], kts[:, half, :], True, True,
                     r=[t_qT[qp], t_kt], w=[t_sps[sp_]])
                P.copy("dve", ssb[tl][:, half, h, :], s_ps[sp_][:, 0:128], r=[t_sps[sp_]], w=[t_ssb[tl]])
        for tl in range(ntb):
            t = b0 + tl
            sl = slice(t * 128, (t + 1) * 128)
            k2 = t % 2
            S = ssb[tl]
            P.dma("sp", s1d[sl], S[:, 0], r=[t_ssb[tl]])
            for h in range(8):
                for half in range(2):
                    src = S[:, half, h, :]
                    P.op("dve", lambda e, src=src, half=half: e.max(out=v16[:, half, 0:8], in_=src),
                         r=[t_ssb[tl]], w=[t_v16])
                    P.op("dve", lambda e, src=src, half=half: e.match_replace(
                        out=tmp, in_to_replace=v16[:, half, 0:8], in_values=src, imm_value=NEG_BIG),
                        r=[t_ssb[tl], t_v16], w=[t_tmp])
                    P.op("dve", lambda e, half=half: e.max(out=v16[:, half, 8:16], in_=tmp),
                         r=[t_tmp], w=[t_v16])
                P.tt("dve", cand.rearrange("p (a b) -> p a b", a=16),
                     v16[:, 0, :].unsqueeze(2).to_broadcast([128, 16, 16]),
                     v16[:, 1, :].unsqueeze(1).to_broadcast([128, 16, 16]), ALU.add,
                     r=[t_v16], w=[t_cand])
                P.op("dve", lambda e, h=h, k2=k2: e.max(out=top[k2][:, h, 0:8], in_=cand),
                     r=[t_cand], w=[t_top[k2]])
                P.op("dve", lambda e, h=h, k2=k2: e.match_replace(
                    out=cand2, in_to_replace=top[k2][:, h, 0:8], in_values=cand, imm_value=NEG_BIG),
                    r=[t_cand, t_top[k2]], w=[t_cand2])
                P.op("dve", lambda e, h=h, k2=k2: e.max(out=top[k2][:, h, 8:16], in_=cand2),
                     r=[t_cand2], w=[t_top[k2]])
            M = sm[k2]
            P.op("dve", lambda e, M=M, k2=k2: e.tensor_reduce(M[:, 0, :], top[k2], AX.X, ALU.max),
                 r=[t_top[k2]], w=[t_sm[k2]])
            P.op("dve", lambda e, M=M, k2=k2: e.tensor_reduce(M[:, 1, :], top[k2], AX.X, ALU.min),
                 r=[t_top[k2]], w=[t_sm[k2]])
            P.ts("dve", M[:, 2, :], M[:, 0, :], -1.0, None, ALU.mult, r=[t_sm[k2]], w=[t_sm[k2]])
            for h in range(8):
                P.act(e16, top[k2][:, h, :], AF.Exp, r=[t_top[k2], t_sm[k2]], w=[t_e16, t_sm[k2]],
                      bias=M[:, 2, h:h + 1], accum_out=M[:, 3, h:h + 1])
            P.act(M[:, 3, :], M[:, 3, :], AF.Ln, r=[t_sm[k2]], w=[t_sm[k2]])
            P.tt("dve", M[:, 4, :], M[:, 2, :], M[:, 3, :], ALU.subtract, r=[t_sm[k2]], w=[t_sm[k2]])
            P.tt("dve", M[:, 5, :], M[:, 1, :], M[:, 4, :], ALU.add, r=[t_sm[k2]], w=[t_sm[k2]])
            P.ts("dve", M[:, 5, :], M[:, 5, :], -THR_MARGIN, None, ALU.add, r=[t_sm[k2]], w=[t_sm[k2]])
            P.tt("dve", s2m[k2], S[:, 1], M[:, 4, :].unsqueeze(2).to_broadcast([128, 8, 128]), ALU.add,
                 r=[t_ssb[tl], t_sm[k2]], w=[t_s2m[k2]])
            P.dma("sp", s2d[sl], s2m[k2], r=[t_s2m[k2]])
            P.dma("sp", thd[sl], M[:, 5, :], r=[t_sm[k2]])
    P.barrier()


def emit_castw(P, src, dst, rows_per=2048):
    R, C = src.shape
    t = P.trk()
    step = max(1, rows_per)
    for r0 in range(0, R, step):
        r1 = min(R, r0 + step)
        for c0 in range(0, C, 2048):
            c1 = min(C, c0 + 2048)
            P.dma("pool", dst[r0:r1, c0:c1], src[r0:r1, c0:c1], w=[t])
    P.barrier()


def emit_peer2(P, nt, hTd, s1d, s2d, thd, uTb, vb, ident, yd):
    TB = 4
    hT = P.sbuf("hT_sb", [128, KC, TB * 128], BF16)
    t_hT = P.trk()
    acc = P.sbuf("acc", [128, TB, D])
    t_acc = P.trks(TB)
    s1b = P.sbuf("s1b", [128, TB, 8, 128])
    s2b = P.sbuf("s2b", [128, TB, 8, 128])
    thb = P.sbuf("thb", [128, TB, 8])
    t_in = P.trk()
    us = [P.sbuf(f"u_sb{i}", [128, KC, 512], BF16) for i in range(2)]
    vs = [P.sbuf(f"v_sb{i}", [128, 4, D], BF16) for i in range(2)]
    t_us = P.trks(2)
    t_vs = P.trks(2)
    S = P.sbuf("S", [128, 8, 4, 128])
    t_S = P.trk()
    E = P.sbuf("E", [128, 8, 4, 128])
    t_E = P.trk()
    Gs = [P.sbuf(f"Gs{i}", [128, 4, 128]) for i in range(2)]
    t_Gs = P.trks(2)
    sqb = P.sbuf("sqb", [128, 4 * 512])
    t_sqb = P.trk()
    gb = P.sbuf("gb", [128, 4 * 512])
    t_gb = P.trk()
    WT = [P.sbuf(f"WT{i}", [128, 4, 128], BF16) for i in range(2)]
    t_WT = P.trks(2)
    idb = P.sbuf("ident_f32", [128, 128])
    t_id = P.trk()
    a_ps = P.psum("a_ps", [128, 4 * 512])
    t_aps = P.ptrk()
    GT_ps = [P.psum(f"GT_ps{i}", [128, 512]) for i in range(2)]
    t_GT = P.ptrks(2)
    o_ps = P.psum("o_ps", [128, 1024])
    t_ops = P.ptrk()

    P.dma("sp", idb, ident, w=[t_id])
    uv = uTb.rearrange("(kc p) e -> p kc e", p=128)
    gb3 = gb.rearrange("p (i t) -> p i t", i=4)
    n = 0
    nw = 0
    for t0 in range(0, nt, TB):
        ntb = min(TB, nt - t0)
        bw = ntb * 128
        rs = slice(t0 * 128, (t0 + ntb) * 128)
        P.dma("sp", hT[:, :, 0:bw], hTd[:, :, rs], w=[t_hT])
        P.dma("sp", s1b[:, 0:ntb], s1d[rs].rearrange("(tl p) h k -> p tl h k", p=128), w=[t_in])
        P.dma("sp", s2b[:, 0:ntb], s2d[rs].rearrange("(tl p) h k -> p tl h k", p=128), w=[t_in])
        P.dma("sp", thb[:, 0:ntb], thd[rs].rearrange("(tl p) h -> p tl h", p=128), w=[t_in])
        for eg in range(32):
            wb = nw % 2
            nw += 1
            es = slice(eg * 512, (eg + 1) * 512)
            for q in range(0, KC, 8):
                P.dma("sp", us[wb][:, q:q + 8, :], uv[:, q:q + 8, es], w=[t_us[wb]])
            P.dma("sp", vs[wb], vb[es, :].rearrange("(i p) n -> p i n", p=128), w=[t_vs[wb]])
            for i in range(4):
                for kc in range(KC):
                    P.mm(a_ps[:, i * 512:i * 512 + bw], us[wb][:, kc, i * 128:(i + 1) * 128], hT[:, kc, 0:bw],
                         kc == 0, kc == KC - 1, r=[t_us[wb], t_hT], w=[t_aps])
            emit_gelu(P, gb, a_ps, sqb, t_aps, t_sqb, t_gb, 4 * 512)
            for tl in range(ntb):
                k = n % 2
                n += 1
                P.tt("pool", S, s1b[:, tl, :, eg * 4:(eg + 1) * 4].unsqueeze(3).to_broadcast([128, 8, 4, 128]),
                     s2b[:, tl].unsqueeze(2).to_broadcast([128, 8, 4, 128]), ALU.add, r=[t_in], w=[t_S])
                P.act(E, S, AF.Exp, r=[t_S], w=[t_E])
                P.tt("dve", S, S, thb[:, tl, :].unsqueeze(2).unsqueeze(3).to_broadcast([128, 8, 4, 128]), ALU.is_ge,
                     r=[t_S, t_in], w=[t_S])
                P.tt("dve", E, S, E, ALU.mult, r=[t_S, t_E], w=[t_E])
                P.op("dve", lambda e, k=k: e.tensor_reduce(Gs[k], E.rearrange("p h i j -> p i j h"), AX.X, ALU.add),
                     r=[t_E], w=[t_Gs[k]])
                for i in range(4):
                    P.tr(GT_ps[k][:, i * 128:(i + 1) * 128], Gs[k][:, i, :], idb, r=[t_Gs[k], t_id], w=[t_GT[k]])
                P.tt("dve", WT[k], gb3[:, :, tl * 128:(tl + 1) * 128],
                     GT_ps[k][:, 0:512].rearrange("p (i j) -> p i j", i=4), ALU.mult,
                     r=[t_gb, t_GT[k]], w=[t_WT[k]])
                for half in range(2):
                    for nbh in range(2):
                        nb = half * 2 + nbh
                        for i in range(4):
                            P.mm(o_ps[:, nbh * 512:(nbh + 1) * 512], WT[k][:, i, :],
                                 vs[wb][:, i, nb * 512:(nb + 1) * 512], i == 0, i == 3,
                                 r=[t_WT[k], t_vs[wb]], w=[t_ops])
                    dst = acc[:, tl, half * 1024:(half + 1) * 1024]
                    if eg == 0:
                        P.copy("act", dst, o_ps, r=[t_ops], w=[t_acc[tl]])
                    else:
                        P.tt("dve", dst, dst, o_ps, ALU.add, r=[t_ops, t_acc[tl]], w=[t_acc[tl]])
        for tl in range(ntb):
            t = t0 + tl
            P.dma("sp", yd[t * 128:(t + 1) * 128, :], acc[:, tl, :], r=[t_acc[tl]])
    P.barrier()


def build_mega(nl, n_lat, n_ctx=2, only=None):
    P = Prog()
    nt = n_lat + n_ctx
    T = nt * 128
    L = n_lat * 128
    ns = (nl + 2) // 3
    I = {}
    I["x"] = P.din("x", [L, D])
    I["ctx"] = P.din("ctx", [n_ctx * 128, D])
    I["cT"] = P.din("cT", [128, KC, 2])
    I["w_mod"] = P.din("w_mod", [nl, D, 6 * D])
    I["b_mod"] = P.din("b_mod", [nl, 6 * D])
    I["norm1_g"] = P.din("norm1_g", [nl, D])
    I["norm2_g"] = P.din("norm2_g", [nl, D])
    I["final_g"] = P.din("final_g", [D])
    I["peer_wq"] = P.din("peer_wq", [nl, D, D])
    I["keysT"] = P.din("keysT", [nl, 2, 128, 128])
    I["peer_uT"] = P.din("peer_uT", [nl, D, 16384])
    I["peer_v"] = P.din("peer_v", [nl, 16384, D])
    I["ident"] = P.din("ident", [128, 128])
    if ns:
        I["wz"] = P.din("wz", [ns, 8, D, 512])
        I["wxbc"] = P.din("wxbc", [ns, 8, D, 768])
        I["wdt"] = P.din("wdt", [ns, 8, D, 16])
        I["cw"] = P.din("cw", [ns, 8, 128, 6, 5])
        I["cb"] = P.din("cb", [ns, 8, 128, 6])
        I["svec"] = P.din("svec", [ns, 8, 3, 16])
        I["ssm_norm_g"] = P.din("ssm_norm_g", [ns, 4096])
        I["ssm_w_out"] = P.din("ssm_w_out", [ns, 4096, D])
        I["cst"] = P.din("cst", [2, 128, 640])
    if nl >= 2:
        I["cv_w1"] = P.din("cv_w1", [D, 2 * D])
        I["cv_vec"] = P.din("cv_vec", [128, 4, 32])
        I["cv_dww"] = P.din("cv_dww", [128, KC, 31])
        I["ones"] = P.din("ones", [128, 128])
        I["cv_w2"] = P.din("cv_w2", [D, D])
        I["cv_b2"] = P.din("cv_b2", [D])
    if nl >= 3:
        I["sg_w_in"] = P.din("sg_w_in", [D, 8192])
        I["sg_b_in"] = P.din("sg_b_in", [1, 8192])
        I["sg_ln"] = P.din("sg_ln", [2, 4096])
        I["sg_wsT"] = P.din("sg_wsT", [8, 128, 128])
        I["sg_bsT"] = P.din("sg_bsT", [128, 8])
        I["sg_w_out"] = P.din("sg_w_out", [4096, D])
        I["sg_b_out"] = P.din("sg_b_out", [D])
    outd = P.dout("out", [L, D])

    xbuf = P.dtmp("xbuf", [T, D])
    hT = P.dtmp("hT", [128, KC, T], BF16)
    ypart = P.dtmp("ypart", [T, D])
    y4096 = P.dtmp("y4096", [T, 4096])
    yT32 = P.dtmp("yT32", [128, 32, T], BF16)
    sT16 = P.dtmp("sT16", [128, KC, T], BF16)
    yf = P.dtmp("yf", [T, 512])
    s1d = P.dtmp("s1", [T, 8, 128])
    s2d = P.dtmp("s2m", [T, 8, 128])
    thd = P.dtmp("thm", [T, 8])
    u_bf = P.dtmp("u_bf", [D, 16384], BF16)
    v_bf = P.dtmp("v_bf", [16384, D], BF16)
    modd = P.dtmp("modd", [nl, 2, 6 * D])

    tiles = list(range(nt))
    ty = lambda t: 1 if t >= n_lat else 0
    xtile = lambda t: xbuf[t * 128:(t + 1) * 128, :]
    ptile = lambda t: ypart[t * 128:(t + 1) * 128, :]

    def x_ext(t):
        if t < n_lat:
            return I["x"][t * 128:(t + 1) * 128, :]
        return I["ctx"][(t - n_lat) * 128:(t - n_lat + 1) * 128, :]

    def mrow(i, r, k):
        return modd[i, r, k * D:(k + 1) * D]

    emit_mod(P, I["cT"], I["w_mod"], I["b_mod"], modd, nl)
    for i in range(nl):
        kind, j = i % 3, i // 3
        rows = [mrow(i - 1, 0, 5) if i else None, mrow(i - 1, 1, 5) if i else None, None, I["norm1_g"][i],
                mrow(i, 0, 1), mrow(i, 1, 1), mrow(i, 0, 0), mrow(i, 1, 0)]
        emit_prep(P, tiles, xtile if i else x_ext, xtile, ptile if i else None, rows, ty, I["ident"], hT_out=hT)
        if kind == 0:
            ctx_g = list(range(n_lat, nt))
            lat_g = list(range(n_lat))
            for g in range(8):
                emit_ssd_group(P, hT, I["wz"][j, g], I["wxbc"][j, g], I["wdt"][j, g], I["cw"][j, g], I["cb"][j, g],
                               I["svec"][j, g], I["cst"],
                               lambda gid, g=g: y4096[gid * 128:(gid + 1) * 128, g * 512:(g + 1) * 512],
                               yf, ctx_g, lat_g)
            rows4 = [None, None, None, I["ssm_norm_g"][j], None, None, None, None]
            emit_prep(P, tiles, lambda t: y4096[t * 128:(t + 1) * 128, :], None, None, rows4, ty, I["ident"],
                      hT_out=yT32, d=4096, plain=True)
            emit_lin(P, nt, yT32, I["ssm_w_out"][j], ypart)
            bias = None
        elif kind == 1:
            for s in range((n_lat + 7) // 8):
                emit_conv_slab(P, hT, s, I["cv_w1"], I["cv_vec"], I["cv_dww"], I["ones"], sT16, n_lat, n_ctx)
            emit_lin(P, nt, sT16, I["cv_w2"], ypart)
            bias = I["cv_b2"]
        else:
            emit_gmlp(P, nt, hT, I["sg_w_in"], I["sg_b_in"], I["sg_ln"], I["sg_wsT"], I["sg_bsT"], I["ident"], yT32)
            emit_lin(P, nt, yT32, I["sg_w_out"], ypart)
            bias = I["sg_b_out"]
        rows = [mrow(i, 0, 2), mrow(i, 1, 2), bias, I["norm2_g"][i],
                mrow(i, 0, 4), mrow(i, 1, 4), mrow(i, 0, 3), mrow(i, 1, 3)]
        emit_prep(P, tiles, xtile, xtile, ptile, rows, ty, I["ident"], hT_out=hT)
        emit_peer1(P, nt, hT, I["peer_wq"][i], I["keysT"][i], s1d, s2d, thd)
        emit_castw(P, I["peer_uT"][i], u_bf)
        emit_castw(P, I["peer_v"][i], v_bf)
        emit_peer2(P, nt, hT, s1d, s2d, thd, u_bf, v_bf, I["ident"], ypart)
    rows = [mrow(nl - 1, 0, 5), mrow(nl - 1, 1, 5), None, I["final_g"], None, None, None, None]
    emit_prep(P, list(range(n_lat)), xtile, None, ptile, rows, ty, I["ident"],
              out_dst=lambda t: outd[t * 128:(t + 1) * 128, :])
    return P.build()


def ssd_group_arrays(w_in, conv_w, conv_b, dt_bias, a_log, d_skip):
    DI = 4096
    d0 = 2 * DI + 2048
    wz, wxbc, wdt, cw, cb, vec = [], [], [], [], [], []
    for g in range(8):
        xc = slice(DI + g * 512, DI + (g + 1) * 512)
        bc = slice(2 * DI + g * 128, 2 * DI + (g + 1) * 128)
        cc = slice(2 * DI + 1024 + g * 128, 2 * DI + 1024 + (g + 1) * 128)
        wz.append(w_in[:, g * 512:(g + 1) * 512])
        wxbc.append(np.concatenate([w_in[:, xc], w_in[:, bc], w_in[:, cc]], axis=1))
        wdt.append(np.concatenate([w_in[:, d0 + g * 8:d0 + (g + 1) * 8],
                                   w_in[:, d0 + 64 + g * 8:d0 + 64 + (g + 1) * 8]], axis=1))
        cs = [slice(g * 512, (g + 1) * 512), slice(DI + g * 128, DI + (g + 1) * 128),
              slice(DI + 1024 + g * 128, DI + 1024 + (g + 1) * 128)]
        cwc = np.concatenate([conv_w[:, c] for c in cs], axis=1)
        cbc = np.concatenate([conv_b[c] for c in cs], axis=0)
        cw.append(cwc.reshape(5, 6, 128).transpose(2, 1, 0))
        cb.append(cbc.reshape(6, 128).T)
        hs = slice(g * 8, (g + 1) * 8)
        vec.append(np.stack([np.concatenate([v[0, hs], v[1, hs]]) for v in (dt_bias, a_log, d_skip)]))
    f = lambda a: np.ascontiguousarray(np.stack(a), dtype=np.float32)
    return f(wz), f(wxbc), f(wdt), f(cw), f(cb), f(vec)


def make_inputs(nl, n_lat, x, c, ctx, c_ctx, w_mod, b_mod, norm1_g, norm2_g, final_g,
                peer_wq, peer_keys, peer_u, peer_v,
                ssm_w_in, ssm_conv_w, ssm_conv_b, ssm_dt_bias, ssm_a_log, ssm_d, ssm_norm_g, ssm_w_out,
                cv_w1, cv_b1, cv_dw_w, cv_dw_b, cv_ln_g, cv_ln_b, cv_w2, cv_b2,
                sg_w_in, sg_b_in, sg_ln_g, sg_ln_b, sg_w_s, sg_b_s, sg_w_out, sg_b_out):
    f = lambda a: np.ascontiguousarray(np.asarray(a, dtype=np.float32))
    ns = (nl + 2) // 3
    sh = {
        "w_mod": f(w_mod[:nl]), "b_mod": f(b_mod[:nl]), "norm1_g": f(norm1_g[:nl]), "norm2_g": f(norm2_g[:nl]),
        "final_g": f(final_g), "peer_wq": f(peer_wq[:nl]),
        "keysT": f(np.asarray(peer_keys[:nl]).transpose(0, 1, 3, 2)),
        "peer_uT": f(np.asarray(peer_u[:nl]).transpose(0, 2, 1)), "peer_v": f(peer_v[:nl]),
        "ident": IDENT,
    }
    if ns:
        parts = [ssd_group_arrays(f(ssm_w_in[j]), f(ssm_conv_w[j]), f(ssm_conv_b[j]), f(ssm_dt_bias[j]),
                                  f(ssm_a_log[j]), f(ssm_d[j])) for j in range(ns)]
        for k, name in enumerate(["wz", "wxbc", "wdt", "cw", "cb", "svec"]):
            sh[name] = np.ascontiguousarray(np.stack([p[k] for p in parts]))
        sh["ssm_norm_g"] = f(ssm_norm_g[:ns])
        sh["ssm_w_out"] = f(ssm_w_out[:ns])
        sh["cst"] = ssd_consts()
    if nl >= 2:
        vec, dww = conv_inputs(f(cv_w1[0]), f(cv_b1[0]), f(cv_dw_w[0]), f(cv_dw_b[0]), f(cv_ln_g[0]), f(cv_ln_b[0]))
        sh.update({"cv_w1": f(cv_w1[0]), "cv_vec": vec, "cv_dww": dww, "ones": np.ones((128, 128), np.float32),
                   "cv_w2": f(cv_w2[0]), "cv_b2": f(cv_b2[0])})
    if nl >= 3:
        sh.update({"sg_w_in": f(sg_w_in[0]), "sg_b_in": f(sg_b_in[0]).reshape(1, -1),
                   "sg_ln": f(np.stack([sg_ln_g[0], sg_ln_b[0]])),
                   "sg_wsT": f(np.asarray(sg_w_s[0]).transpose(0, 2, 1)), "sg_bsT": f(np.asarray(sg_b_s[0]).T),
                   "sg_w_out": f(sg_w_out[0]), "sg_b_out": f(sg_b_out[0])})
    ims = []
    for b in range(x.shape[0]):
        im = dict(sh)
        im["x"] = f(x[b][:n_lat * 128])
        im["ctx"] = f(ctx[b])
        cvec = np.stack([np.asarray(c[b], np.float32), np.asarray(c_ctx, np.float32)])
        im["cT"] = np.ascontiguousarray(cvec.reshape(2, KC, 128).transpose(2, 1, 0))
        ims.append(im)
    return ims


def kernel(**inputs):
    x = np.asarray(inputs["x"])
    nl = int(np.asarray(inputs["w_mod"]).shape[0])
    n_lat = x.shape[1] // 128
    ims = make_inputs(nl, n_lat, **inputs)
    nc = build_mega(nl, n_lat, n_ctx=np.asarray(inputs["ctx"]).shape[1] // 128)
    res = run(nc, ims, "mega")
    return np.stack([r["out"] for r in res]).astype(np.float32)
```
